# Optimizing a Trainium2 kernel written in Bass

```python
import math
import jax
import jax.numpy as jnp
from jax import lax
import numpy as np

D_MODEL = 4096
BATCH = 4
SEQ = 2048
DEPTH = 2

N_META = 16
BLOCK = 128
MLA_HEADS = 16
MLA_Q_RANK = 1024
MLA_KV_RANK = 512
MLA_NOPE = 128
MLA_ROPE = 64
MLA_V = 128
ROPE_THETA = 10000.0
SWA_HEADS = 32
SWA_KV_HEADS = 8
SWA_HEAD_DIM = 64
WINDOW = 128
REL_BUCKETS = 32
REL_MAX_DIST = 128
DENSE_FF = 11008
N_EXPERTS = 8
TOP_K = 2
EXPERT_FF = 5632
N_DENSE = (DEPTH + 1) // 2
N_MOE = DEPTH // 2
N_BRANCH = 2
EPS = 1e-6
NEG_INF = -1e30
IN_SPLIT_SIZES = (MLA_Q_RANK, MLA_KV_RANK, MLA_ROPE, SWA_HEADS * SWA_HEAD_DIM,
                  SWA_KV_HEADS * SWA_HEAD_DIM, SWA_KV_HEADS * SWA_HEAD_DIM, D_MODEL, D_MODEL)
IN_COLS = (MLA_Q_RANK + MLA_KV_RANK + MLA_ROPE + SWA_HEADS * SWA_HEAD_DIM
           + 2 * SWA_KV_HEADS * SWA_HEAD_DIM + N_BRANCH * D_MODEL)

kernel_name = 'hybrid_mla_swa_moe_block'


def rms_norm(x, g):
    xf = x.astype(jnp.float32)
    y = xf * lax.rsqrt(jnp.mean(xf * xf, axis=-1, keepdims=True) + EPS)
    return (y * g.astype(jnp.float32)).astype(x.dtype)


def rope(x, pos):
    half = x.shape[-1] // 2
    inv_freq = ROPE_THETA ** (-jnp.arange(half, dtype=jnp.float32) / half)
    ang = pos.astype(jnp.float32)[:, None] * inv_freq[None, :]
    cos = jnp.cos(ang)[:, None, :]
    sin = jnp.sin(ang)[:, None, :]
    xf = x.astype(jnp.float32)
    x1, x2 = xf[..., :half], xf[..., half:]
    return jnp.concatenate([x1 * cos - x2 * sin, x2 * cos + x1 * sin], axis=-1).astype(x.dtype)


def t5_bucket(rel):
    n = jnp.maximum(rel, 0)
    max_exact = REL_BUCKETS // 2
    nf = jnp.maximum(n, 1).astype(jnp.float32)
    large = max_exact + (jnp.log(nf / max_exact) / math.log(REL_MAX_DIST / max_exact)
                         * (REL_BUCKETS - max_exact)).astype(jnp.int32)
    large = jnp.minimum(large, REL_BUCKETS - 1)
    return jnp.where(n < max_exact, n, large)


def causal_block_attention(q, k, v, scale):
    L = q.shape[1]
    starts = [0] + list(range(N_META, L, BLOCK))
    ends = starts[1:] + [L]
    outs = []
    for s, e in zip(starts, ends):
        logits = jnp.einsum('bqhd,bkhd->bhqk', q[:, s:e], k[:, :e]).astype(jnp.float32) * scale
        mask = jnp.arange(s, e)[:, None] >= jnp.arange(e)[None, :]
        logits = jnp.where(mask, logits, NEG_INF)
        probs = jax.nn.softmax(logits, axis=-1).astype(v.dtype)
        outs.append(jnp.einsum('bhqk,bkhd->bqhd', probs, v[:, :e]))
    return jnp.concatenate(outs, axis=1)


def mla_branch(c_q, c_kv, k_pe, cq_g, ckv_g, w_uq, w_ukv, qn_g, kn_g):
    B, L = c_q.shape[0], c_q.shape[1]
    q = (rms_norm(c_q, cq_g) @ w_uq).reshape(B, L, MLA_HEADS, MLA_NOPE + MLA_ROPE)
    kv = (rms_norm(c_kv, ckv_g) @ w_ukv).reshape(B, L, MLA_HEADS, MLA_NOPE + MLA_V)
    k_nope, v = kv[..., :MLA_NOPE], kv[..., MLA_NOPE:]
    k = jnp.concatenate([k_nope, jnp.broadcast_to(k_pe[:, :, None, :], (B, L, MLA_HEADS, MLA_ROPE))], axis=-1)
    q = rms_norm(q, qn_g)
    k = rms_norm(k, kn_g)
    pos = jnp.arange(L)
    q = jnp.concatenate([q[..., :MLA_NOPE], rope(q[..., MLA_NOPE:], pos)], axis=-1)
    k = jnp.concatenate([k[..., :MLA_NOPE], rope(k[..., MLA_NOPE:], pos)], axis=-1)
    out = causal_block_attention(q, k, v, (MLA_NOPE + MLA_ROPE) ** -0.5)
    return out.reshape(B, L, MLA_HEADS * MLA_V)


def swa_branch(q, k, v, sinks, rel_bias):
    B, L = q.shape[0], q.shape[1]
    pad = (-N_META) % BLOCK
    tail = (-(L + pad)) % BLOCK
    nb = (L + pad + tail) // BLOCK
    group = SWA_HEADS // SWA_KV_HEADS

    def to_blocks(t):
        t = jnp.pad(t, ((0, 0), (pad, tail), (0, 0), (0, 0)))
        return t.reshape((B, nb, BLOCK) + t.shape[2:])

    def band(t):
        prev = jnp.concatenate([jnp.zeros_like(t[:, :1]), t[:, :-1]], axis=1)
        return jnp.concatenate([prev, t], axis=2)

    qb = to_blocks(q).reshape(B, nb, BLOCK, SWA_KV_HEADS, group, SWA_HEAD_DIM)
    kk, vv = band(to_blocks(k)), band(to_blocks(v))
    qi = jnp.arange(BLOCK)[:, None]
    sj = jnp.arange(2 * BLOCK)[None, :]
    rel = qi + BLOCK - sj
    kpos = jnp.arange(nb)[:, None, None] * BLOCK - BLOCK + sj[None]
    valid = (rel >= 0)[None] & (rel < WINDOW)[None] & (kpos >= pad) & (kpos < pad + L)
    bias = rel_bias[t5_bucket(rel)].astype(jnp.float32)
    bias = jnp.transpose(bias, (2, 0, 1)).reshape(SWA_KV_HEADS, group, BLOCK, 2 * BLOCK)
    logits = jnp.einsum('bnqhgd,bnshd->bnhgqs', qb, kk).astype(jnp.float32) * (SWA_HEAD_DIM ** -0.5) + bias
    logits = jnp.where(valid[None, :, None, None], logits, NEG_INF)
    sink = jnp.broadcast_to(sinks.astype(jnp.float32).reshape(1, 1, SWA_KV_HEADS, group, 1, 1),
                            logits.shape[:-1] + (1,))
    probs = jax.nn.softmax(jnp.concatenate([logits, sink], axis=-1), axis=-1)[..., :-1]
    out = jnp.einsum('bnhgqs,bnshd->bnqhgd', probs.astype(v.dtype), vv)
    return out.reshape(B, nb * BLOCK, SWA_HEADS * SWA_HEAD_DIM)[:, pad:pad + L]


def swiglu(h, w1, w3, w2):
    return (jax.nn.silu(h @ w1) * (h @ w3)) @ w2


def moe_swiglu(h, router, w1, w3, w2):
    logits = (h @ router).astype(jnp.float32)
    top_vals, top_idx = lax.top_k(logits, TOP_K)
    top_w = jax.nn.softmax(top_vals, axis=-1)
    gate = jnp.sum(jax.nn.one_hot(top_idx, N_EXPERTS, dtype=jnp.float32) * top_w[..., None], axis=-2).astype(h.dtype)
    out = jnp.zeros_like(h)
    for e in range(N_EXPERTS):
        out = out + gate[..., e:e + 1] * swiglu(h, w1[e], w3[e], w2[e])
    return out


def setup_inputs(seed: int = 0) -> dict:
    key = jax.random.key(seed)
    ks = iter(jax.random.split(key, 32))

    def nrm(shape, scale):
        return jax.random.normal(next(ks), shape, jnp.float32) * scale

    def gain(shape):
        return 1.0 + 0.02 * jax.random.normal(next(ks), shape, jnp.float32)

    D = D_MODEL
    return {
        'x': nrm((BATCH, SEQ, D), 1.0),
        'meta_tokens': nrm((N_META, D), 1.0),
        'rel_bias': nrm((REL_BUCKETS, SWA_HEADS), 0.5),
        'attn_norm': gain((DEPTH, D)),
        'w_in': nrm((DEPTH, D, IN_COLS), D ** -0.5),
        'mla_cq_norm': gain((DEPTH, MLA_Q_RANK)),
        'mla_ckv_norm': gain((DEPTH, MLA_KV_RANK)),
        'mla_w_uq': nrm((DEPTH, MLA_Q_RANK, MLA_HEADS * (MLA_NOPE + MLA_ROPE)), MLA_Q_RANK ** -0.5),
        'mla_w_ukv': nrm((DEPTH, MLA_KV_RANK, MLA_HEADS * (MLA_NOPE + MLA_V)), MLA_KV_RANK ** -0.5),
        'mla_q_norm': gain((DEPTH, MLA_NOPE + MLA_ROPE)),
        'mla_k_norm': gain((DEPTH, MLA_NOPE + MLA_ROPE)),
        'swa_q_norm': gain((DEPTH, SWA_HEAD_DIM)),
        'swa_k_norm': gain((DEPTH, SWA_HEAD_DIM)),
        'swa_sinks': nrm((DEPTH, SWA_HEADS), 1.0),
        'w_branch_mla': nrm((DEPTH, MLA_HEADS * MLA_V, D), (MLA_HEADS * MLA_V) ** -0.5),
        'w_branch_swa': nrm((DEPTH, SWA_HEADS * SWA_HEAD_DIM, D), (SWA_HEADS * SWA_HEAD_DIM) ** -0.5),
        'w_out': nrm((DEPTH, D, D), D ** -0.5),
        'ffn_norm': gain((DEPTH, D)),
        'dense_w1': nrm((N_DENSE, D, DENSE_FF), D ** -0.5),
        'dense_w3': nrm((N_DENSE, D, DENSE_FF), D ** -0.5),
        'dense_w2': nrm((N_DENSE, DENSE_FF, D), DENSE_FF ** -0.5),
        'moe_router': nrm((N_MOE, D, N_EXPERTS), D ** -0.5),
        'moe_w1': nrm((N_MOE, N_EXPERTS, D, EXPERT_FF), D ** -0.5),
        'moe_w3': nrm((N_MOE, N_EXPERTS, D, EXPERT_FF), D ** -0.5),
        'moe_w2': nrm((N_MOE, N_EXPERTS, EXPERT_FF, D), EXPERT_FF ** -0.5),
    }


def reference(x, meta_tokens, rel_bias, attn_norm, w_in, mla_cq_norm, mla_ckv_norm, mla_w_uq,
              mla_w_ukv, mla_q_norm, mla_k_norm, swa_q_norm, swa_k_norm, swa_sinks,
              w_branch_mla, w_branch_swa, w_out, ffn_norm, dense_w1, dense_w3, dense_w2,
              moe_router, moe_w1, moe_w3, moe_w2):
    B = x.shape[0]
    meta = jnp.broadcast_to(meta_tokens.astype(x.dtype)[None], (B, N_META, D_MODEL))
    h_res = jnp.concatenate([meta, x], axis=1)
    L = h_res.shape[1]
    offsets = []
    acc = 0
    for s in IN_SPLIT_SIZES[:-1]:
        acc += s
        offsets.append(acc)
    for i in range(DEPTH):
        h = rms_norm(h_res, attn_norm[i])
        proj = h @ w_in[i]
        c_q, c_kv, k_pe, q_s, k_s, v_s, g_a, g_b = jnp.split(proj, offsets, axis=-1)
        a = mla_branch(c_q, c_kv, k_pe, mla_cq_norm[i], mla_ckv_norm[i], mla_w_uq[i],
                       mla_w_ukv[i], mla_q_norm[i], mla_k_norm[i])
        q_s = rms_norm(q_s.reshape(B, L, SWA_HEADS, SWA_HEAD_DIM), swa_q_norm[i])
        k_s = rms_norm(k_s.reshape(B, L, SWA_KV_HEADS, SWA_HEAD_DIM), swa_k_norm[i])
        v_s = v_s.reshape(B, L, SWA_KV_HEADS, SWA_HEAD_DIM)
        b = swa_branch(q_s, k_s, v_s, swa_sinks[i], rel_bias)
        merged = jax.nn.sigmoid(g_a) * (a @ w_branch_mla[i]) + jax.nn.sigmoid(g_b) * (b @ w_branch_swa[i])
        h_res = h_res + merged @ w_out[i]
        h = rms_norm(h_res, ffn_norm[i])
        if i % 2 == 0:
            f = swiglu(h, dense_w1[i // 2], dense_w3[i // 2], dense_w2[i // 2])
        else:
            f = moe_swiglu(h, moe_router[i // 2], moe_w1[i // 2], moe_w3[i // 2], moe_w2[i // 2])
        h_res = h_res + f
    return h_res[:, N_META:]
```

```python
import math
from contextlib import ExitStack
import numpy as np
import ml_dtypes
import concourse.bass as bass
import concourse.mybir as mybir
from concourse.bass_utils import run_bass_kernel_spmd

F32 = mybir.dt.float32
BF16 = mybir.dt.bfloat16
AF = mybir.ActivationFunctionType
ALU = mybir.AluOpType
EPS = 1e-6


class Cfg:
    def __init__(self, D=4096, S=2048, G=1024, HM=16, QR=1024, KVR=512, HS=32, HKV=8,
                 FF=11008, EFF=5632, NE=8, DEPTH=2, FT=516, B=4):
        self.D, self.S, self.G, self.HM, self.QR, self.KVR = D, S, G, HM, QR, KVR
        self.HS, self.HKV, self.FF, self.EFF, self.NE, self.DEPTH, self.FT, self.B = HS, HKV, FF, EFF, NE, DEPTH, FT, B
        self.NM = 16
        self.L = self.NM + S
        self.DC = D // 128
        ng = S // G
        self.groups = [(i * G, G if i < ng - 1 else G + self.NM) for i in range(ng)]
        self.o_cq = 0
        self.o_ckv = QR
        self.o_kpe = QR + KVR
        self.o_qs = self.o_kpe + 64
        self.o_ks = self.o_qs + HS * 64
        self.o_vs = self.o_ks + HKV * 64
        self.o_ga = self.o_vs + HKV * 64
        self.o_gb = self.o_ga + D
        self.INC = self.o_gb + D
        self.NDENSE = (DEPTH + 1) // 2
        self.NMOE = DEPTH // 2


def tchunks(T, mx=512):
    n = (T + mx - 1) // mx
    base = ((T + n - 1) // n + 15) // 16 * 16
    out, s = [], 0
    while s < T:
        w = min(base, T - s)
        out.append((s, w))
        s += w
    return out


class Buf:
    _n = 0

    def __init__(self, name=""):
        self.w = {}
        self.r = {}
        self.name = name
        Buf._n += 1
        self.uid = Buf._n


class KB:
    def __init__(self, nc, stack):
        self.nc = nc
        self.eng = {'pe': nc.tensor, 'act': nc.scalar, 'dve': nc.vector, 'pool': nc.gpsimd, 'sp': nc.sync}
        self.esem = {e: stack.enter_context(nc.semaphore('s_' + e)) for e in ['pe', 'act', 'dve', 'pool']}
        self.ecnt = {e: 0 for e in self.esem}
        self.waited = {}
        self.dsem = [stack.enter_context(nc.semaphore('d%d' % i)) for i in range(40)]
        self.dcnt = [0] * len(self.dsem)
        self.dfree = list(range(len(self.dsem)))
        self.slot_sem = {}

    def _wait(self, e, key, h, val):
        k = (e, key)
        if self.waited.get(k, 0) >= val:
            return
        if key == 'E' + e and (e == 'pe' or val > self.ecnt[e]):
            return
        self.eng[e].wait_ge(h, val)
        self.waited[k] = val
        if getattr(self, 'log', None) is not None:
            self.log.append(('wait', e, key, val))

    def _deps(self, e, reads, writes, partial):
        for b in reads:
            for key, (h, val) in b.w.items():
                self._wait(e, key, h, val)
        for b in writes:
            if not partial:
                for key, (h, val) in b.w.items():
                    self._wait(e, key, h, val)
            for key, (h, val) in b.r.items():
                self._wait(e, key, h, val)

    def _record(self, key, h, val, reads, writes, partial):
        for b in reads:
            b.r[key] = (h, val)
        for b in writes:
            if partial:
                b.w[key] = (h, val)
            else:
                b.w = {key: (h, val)}
                b.r = {}

    def op(self, e, fn, reads=(), writes=(), signal=True):
        self._deps(e, reads, writes, False)
        ins = fn(self.eng[e])
        h = self.esem[e]
        val = self.ecnt[e] + 1
        if signal:
            ins.then_inc(h, 1)
            self.ecnt[e] = val
        if getattr(self, 'log', None) is not None:
            self.log.append(('op', e, val, signal))
        self._record('E' + e, h, val, reads, writes, False)
        return ins

    def slot(self, buf):
        if buf.uid not in self.slot_sem:
            self.slot_sem[buf.uid] = self.dfree.pop(0)
        return self.slot_sem[buf.uid]

    def dma(self, q, out, in_, sb, reads=(), writes=(), partial=False, **kw):
        self._deps(q, reads, writes, partial)
        i = self.slot(sb)
        ins = self.eng[q].dma_start(out=out, in_=in_, **kw)
        ins.then_inc(self.dsem[i], 16)
        self.dcnt[i] += 16
        if getattr(self, 'log', None) is not None:
            self.log.append(('dma', q, 'D%d' % i, self.dcnt[i], sb.name))
        self._record('D%d' % i, self.dsem[i], self.dcnt[i], reads, writes, partial)
        return ins

    def barrier(self):
        for e in self.eng:
            for p in self.esem:
                if self.ecnt[p] > 0 and p != e:
                    self._wait(e, 'E' + p, self.esem[p], self.ecnt[p])
            for i in range(len(self.dsem)):
                if self.dcnt[i] > 0:
                    self._wait(e, 'D%d' % i, self.dsem[i], self.dcnt[i])
        self.slot_sem = {}
        self.dfree = list(range(len(self.dsem)))


def build(cfg, dbg=()):
    nc = bass.Bass("TRN2", target_bir_lowering=False)
    c = cfg
    D, L, DC = c.D, c.L, c.DC
    with ExitStack() as top:
        kb = KB(nc, top)

        def dram_in(name, shape, dt=F32):
            return nc.dram_tensor(name, list(shape), dt, kind="ExternalInput").ap()

        x = dram_in("x", [c.S, D])
        meta = dram_in("meta_tokens", [c.NM, D])
        rel_bias = dram_in("rel_bias", [32, c.HS])
        attn_norm = dram_in("attn_norm", [c.DEPTH, D])
        w_in = dram_in("w_in", [c.DEPTH, D, c.INC])
        cq_norm = dram_in("mla_cq_norm", [c.DEPTH, c.QR])
        ckv_norm = dram_in("mla_ckv_norm", [c.DEPTH, c.KVR])
        w_uq = dram_in("mla_w_uq", [c.DEPTH, c.QR, c.HM * 192])
        w_ukv = dram_in("mla_w_ukv", [c.DEPTH, c.KVR, c.HM * 256])
        q_norm = dram_in("mla_q_norm", [c.DEPTH, 192])
        k_norm = dram_in("mla_k_norm", [c.DEPTH, 192])
        sq_norm = dram_in("swa_q_norm", [c.DEPTH, 64])
        sk_norm = dram_in("swa_k_norm", [c.DEPTH, 64])
        sinks = dram_in("swa_sinks", [c.DEPTH, c.HS])
        w_bm = dram_in("w_branch_mla", [c.DEPTH, c.HM * 128, D])
        w_bs = dram_in("w_branch_swa", [c.DEPTH, c.HS * 64, D])
        w_out = dram_in("w_out", [c.DEPTH, D, D])
        ffn_norm = dram_in("ffn_norm", [c.DEPTH, D])
        d_w1 = dram_in("dense_w1", [c.NDENSE, D, c.FF])
        d_w3 = dram_in("dense_w3", [c.NDENSE, D, c.FF])
        d_w2 = dram_in("dense_w2", [c.NDENSE, c.FF, D])
        router = dram_in("moe_router", [c.NMOE, D, c.NE])
        m_w1 = dram_in("moe_w1", [c.NMOE, c.NE, D, c.EFF])
        m_w3 = dram_in("moe_w3", [c.NMOE, c.NE, D, c.EFF])
        m_w2 = dram_in("moe_w2", [c.NMOE, c.NE, c.EFF, D])
        c_ident = dram_in("c_ident", [128, 128])
        c_tri = dram_in("c_tri", [128, 512])
        c_cos = dram_in("c_cos", [64, L])
        c_sin = dram_in("c_sin", [64, L])
        c_prot = dram_in("c_prot", [64, 64])
        c_oh = dram_in("c_oh", [32, 128])
        c_anti = dram_in("c_anti", [128, 384])
        out = nc.dram_tensor("out", [c.S, D], F32, kind="ExternalOutput").ap()

        def scratch(name, shape, dt):
            if name in dbg:
                return nc.dram_tensor(name, list(shape), dt, kind="ExternalOutput").ap()
            return nc.dram_tensor(name, list(shape), dt).ap()

        hres = scratch("hres", [D, L], F32)
        proj = scratch("proj", [c.INC, L], BF16)
        qT = scratch("qT", [c.HM * 192, L], BF16)
        kT = scratch("kT", [c.HM * 192, L], BF16)
        vM = scratch("vM", [L, c.HM * 128], BF16)
        vS = scratch("vS", [L, c.HKV * 64], BF16)
        aT = scratch("aT", [c.HM * 128, L], BF16)
        bT = scratch("bT", [c.HS * 64, L], BF16)
        mg = scratch("mg", [D, L], BF16)
        dbg_hn = scratch("dbg_hn", [D, L], BF16) if 'dbg_hn' in dbg else None
        dbg_hid = scratch("dbg_hid", [max(c.FF, c.EFF), L], BF16) if 'dbg_hid' in dbg else None
        B_hres, B_proj, B_qT, B_kT, B_vM, B_vS, B_aT, B_bT, B_mg, B_out = [Buf(n) for n in
            ["hres", "proj", "qT", "kT", "vM", "vS", "aT", "bT", "mg", "out"]]

        banks = [top.enter_context(nc.psum_tensor("pb%d" % i, [128, 512], F32)) for i in range(8)]
        Bb = [Buf("bank%d" % i) for i in range(8)]

        _uid = [0]

        def sb(stack, name, shape, dt):
            _uid[0] += 1
            return stack.enter_context(nc.sbuf_tensor("%s_%d" % (name, _uid[0]), list(shape), dt))

        ident = sb(top, "ident", [128, 128], F32)
        ones32 = sb(top, "ones32", [128, 128], F32)
        onesbf = sb(top, "onesbf", [128, 128], BF16)
        tri = sb(top, "tri", [128, 512], BF16)
        B_const = Buf("const")
        kb.dma('sp', ident[:], c_ident[:, :], B_const, writes=[B_const], partial=True)
        kb.dma('pool', tri[:], c_tri[:, :], B_const, writes=[B_const], partial=True)
        kb.op('dve', lambda e: e.memset(ones32[:], 1.0), writes=[B_const])
        B_ones = Buf("ones")
        kb.op('dve', lambda e: e.memset(onesbf[:], 1.0), writes=[B_ones])
        kb.barrier()

        def rstd_bc(st, parts, dim, T, dst, dstB, bankset):
            tcs = tchunks(T)
            for pi, (src, srcB, kp) in enumerate(parts):
                sq = st['sq'][pi % 2]
                sqB = st['sqB'][pi % 2]
                kb.op('act', lambda e: e.activation(sq[0:kp, 0:T], src, AF.Square), reads=srcB, writes=[sqB])
                for ti, (t0, tw) in enumerate(tcs):
                    bi = bankset[ti]
                    kb.op('pe', lambda e: e.matmul(banks[bi][:, 0:tw], ones32[0:kp, :], sq[0:kp, t0:t0 + tw],
                                                   start=(pi == 0), stop=(pi == len(parts) - 1)),
                          reads=[sqB, B_const], writes=[Bb[bi]], signal=True)
            for ti, (t0, tw) in enumerate(tcs):
                bi = bankset[ti]
                kb.op('act', lambda e: e.activation(dst[:, t0:t0 + tw], banks[bi][:, 0:tw], AF.Sqrt,
                                                    bias=EPS, scale=1.0 / dim), reads=[Bb[bi]], writes=[dstB])
            kb.op('dve', lambda e: e.reciprocal(dst[:, 0:T], dst[:, 0:T]), reads=[dstB], writes=[dstB])

        def phase_in():
            with ExitStack() as st:
                xt = [sb(st, "xt%d" % i, [128, D], F32) for i in range(2)]
                xtB = [Buf() for _ in range(2)]
                ot = [sb(st, "ot%d" % i, [128, 4, 128], F32) for i in range(2)]
                otB = [Buf() for _ in range(2)]
                ntile = (L + 127) // 128
                k = 0
                for tt in range(ntile):
                    t0 = tt * 128
                    tw = min(128, L - t0)
                    xb, xB = xt[tt % 2], xtB[tt % 2]
                    if t0 < c.NM:
                        kb.dma('sp', xb[0:c.NM, :], meta[:, :], xB, writes=[xB])
                        kb.dma('sp', xb[c.NM:tw, :], x[0:tw - c.NM, :], xB, writes=[xB], partial=True)
                    else:
                        kb.dma('sp', xb[0:tw, :], x[t0 - c.NM:t0 - c.NM + tw, :], xB, writes=[xB])
                    for c4 in range(DC // 4):
                        bi = k % 8
                        for j in range(4):
                            cc = c4 * 4 + j
                            kb.op('pe', lambda e: e.transpose(banks[bi][:, j * 128:j * 128 + tw], xb[0:tw, cc * 128:(cc + 1) * 128], ident[0:tw, 0:tw]),
                                  reads=[xB, B_const], writes=[Bb[bi]] if j == 0 else [], signal=(j == 3))
                        o, oB = ot[k % 2], otB[k % 2]
                        eng = 'act' if k % 2 == 0 else 'dve'
                        if eng == 'act':
                            kb.op('act', lambda e: e.copy(o[:, :, 0:tw], banks[bi][:, :].rearrange("p (j t) -> p j t", j=4)[:, :, 0:tw]), reads=[Bb[bi]], writes=[oB])
                        else:
                            kb.op('dve', lambda e: e.tensor_copy(o[:, :, 0:tw], banks[bi][:, :].rearrange("p (j t) -> p j t", j=4)[:, :, 0:tw]), reads=[Bb[bi]], writes=[oB])
                        kb.dma('sp', hres[c4 * 512:(c4 + 1) * 512, t0:t0 + tw].rearrange("(j p) t -> p j t", p=128), o[:, :, 0:tw], oB,
                               reads=[oB], writes=[B_hres], partial=True)
                        k += 1
            kb.barrier()

        phase_in()


        Ecur_d = scratch("Ecur", [128, c.HS * 128], BF16)
        Eprev_d = scratch("Eprev", [128, c.HS * 128], BF16)
        B_E = Buf("E")

        def evcopy(k, out_ap, in_ap, reads, writes):
            if k % 2 == 0:
                kb.op('act', lambda e: e.copy(out_ap, in_ap), reads=reads, writes=writes)
            else:
                kb.op('dve', lambda e: e.tensor_copy(out_ap, in_ap), reads=reads, writes=writes)

        def mk_stats(st, T):
            return {'sq': [sb(st, "sq%d" % i, [128, T], F32) for i in range(2)], 'sqB': [Buf() for _ in range(2)]}

        def col_vec(st, name, src_row, n0, n, P=128):
            cn = n // P
            t = sb(st, name, [P, cn], F32)
            tB = Buf()
            kb.dma('sp', t[:, :], src_row[n0:n0 + n].rearrange("(c p) -> p c", p=P), tB, writes=[tB], allow_slow_non_contiguous=True)
            return t, tB

        def norm_load(st, stats, gain_row, g0, T, hn, hnB, f32cb=None):
            gt, gtB = col_vec(st, "gain", gain_row, 0, D)
            xs = [sb(st, "x32_%d" % i, [128, T], F32) for i in range(2)]
            xsB = [Buf() for _ in range(2)]
            rstd = sb(st, "rstd", [128, T], F32)
            rB = Buf()
            tcs = tchunks(T)
            for cc in range(DC):
                xx, xB = xs[cc % 2], xsB[cc % 2]
                kb.dma('sp', xx[:, :], hres[cc * 128:(cc + 1) * 128, g0:g0 + T], xB, reads=[B_hres], writes=[xB])
                sq, sqB = stats['sq'][cc % 2], stats['sqB'][cc % 2]
                kb.op('act', lambda e: e.activation(sq[:, 0:T], xx[:, :], AF.Square), reads=[xB], writes=[sqB])
                for ti, (t0, tw) in enumerate(tcs):
                    kb.op('pe', lambda e: e.matmul(banks[ti][:, 0:tw], ones32[:, :], sq[:, t0:t0 + tw], start=(cc == 0), stop=(cc == DC - 1)),
                          reads=[sqB, B_const], writes=[Bb[ti]])
            for ti, (t0, tw) in enumerate(tcs):
                kb.op('act', lambda e: e.activation(rstd[:, t0:t0 + tw], banks[ti][:, 0:tw], AF.Sqrt, bias=EPS, scale=1.0 / D), reads=[Bb[ti]], writes=[rB])
            kb.op('dve', lambda e: e.reciprocal(rstd[:, :], rstd[:, :]), reads=[rB], writes=[rB])
            for cc in range(DC):
                xx, xB = xs[cc % 2], xsB[cc % 2]
                kb.dma('sp', xx[:, :], hres[cc * 128:(cc + 1) * 128, g0:g0 + T], xB, reads=[B_hres], writes=[xB])
                if f32cb is not None:
                    f32cb(cc, xx, xB, gt, gtB, rstd, rB)
                kb.op('dve', lambda e: e.scalar_tensor_tensor(out=hn[:, cc, :], in0=xx[:, :], scalar=gt[:, cc:cc + 1], in1=rstd[:, :], op0=ALU.mult, op1=ALU.mult),
                      reads=[xB, gtB, rB], writes=[hnB])

        WB = 16384

        def gemm(st, wb, wbB, xs, xsB, KC, T, W2d, groups, evac, accsets=((0, 1, 2), (3, 4, 5)), xs2=None, W2d2=None, KC2=0):
            tcs = tchunks(T)
            ci = 0
            for gi, (col0, gw) in enumerate(groups):
                w_, wB = wb[gi % 2], wbB[gi % 2]
                wv = w_[:, 0:KC * gw].rearrange("p (k n) -> p k n", n=gw)
                first = True
                for k0 in range(0, KC, 16):
                    k1 = min(KC, k0 + 16)
                    kb.dma('pool', wv[:, k0:k1, :], W2d[k0 * 128:k1 * 128, col0:col0 + gw].rearrange("(k p) n -> p k n", p=128), wB,
                           writes=[wB], partial=not first)
                    first = False
                if W2d2 is not None:
                    wv2 = w_[:, KC * gw:(KC + KC2) * gw].rearrange("p (k n) -> p k n", n=gw)
                    for k0 in range(0, KC2, 16):
                        k1 = min(KC2, k0 + 16)
                        kb.dma('pool', wv2[:, k0:k1, :], W2d2[k0 * 128:k1 * 128, col0:col0 + gw].rearrange("(k p) n -> p k n", p=128), wB,
                               writes=[wB], partial=True)
                for n0 in range(0, gw, 128):
                    mw = min(128, gw - n0)
                    if W2d2 is None:
                        acc = accsets[ci % len(accsets)]
                        for kc in range(KC):
                            for ti, (t0, tw) in enumerate(tcs):
                                kb.op('pe', lambda e: e.matmul(banks[acc[ti]][0:mw, 0:tw], wv[:, kc, n0:n0 + mw], xs[:, kc, t0:t0 + tw], start=(kc == 0), stop=(kc == KC - 1)),
                                      reads=[wB, xsB], writes=[Bb[acc[ti]]], signal=(kc == KC - 1 and ti == len(tcs) - 1))
                        evac(ci, col0 + n0, mw, acc, None)
                    else:
                        if len(tcs) <= 2:
                            accA, accB = (((0, 1), (2, 3)), ((4, 5), (6, 7)))[ci % 2]
                        else:
                            accA, accB = accsets[0], accsets[1]
                        for kc in range(KC):
                            for ti, (t0, tw) in enumerate(tcs):
                                kb.op('pe', lambda e: e.matmul(banks[accA[ti]][0:mw, 0:tw], wv[:, kc, n0:n0 + mw], xs[:, kc, t0:t0 + tw], start=(kc == 0), stop=(kc == KC - 1)),
                                      reads=[wB, xsB], writes=[Bb[accA[ti]]], signal=(kc == KC - 1 and ti == len(tcs) - 1))
                        for kc in range(KC2):
                            for ti, (t0, tw) in enumerate(tcs):
                                kb.op('pe', lambda e: e.matmul(banks[accB[ti]][0:mw, 0:tw], wv2[:, kc, n0:n0 + mw], xs2[:, kc, t0:t0 + tw], start=(kc == 0), stop=(kc == KC2 - 1)),
                                      reads=[wB, xsB], writes=[Bb[accB[ti]]], signal=(kc == KC2 - 1 and ti == len(tcs) - 1))
                        evac(ci, col0 + n0, mw, accA, accB)
                    ci += 1

        def split_groups(ranges, gw=512):
            out_ = []
            for (a, n) in ranges:
                o = a
                while o < a + n:
                    w = min(gw, a + n - o)
                    out_.append((o, w))
                    o += w
            return out_

        def phase_A(i, g0, T):
            with ExitStack() as st:
                stats = mk_stats(st, T)
                hn = sb(st, "hn", [128, DC, T], BF16)
                hnB = Buf()
                norm_load(st, stats, attn_norm[i], g0, T, hn, hnB)
                wb = [sb(st, "wb%d" % j, [128, WB], BF16) for j in range(2)]
                wbB = [Buf() for _ in range(2)]
                stg = [sb(st, "stg%d" % j, [128, T], BF16) for j in range(2)]
                stgB = [Buf() for _ in range(2)]
                tcs = tchunks(T)

                def evac(ci, col, mw, acc, _):
                    s_, sB = stg[ci % 2], stgB[ci % 2]
                    for ti, (t0, tw) in enumerate(tcs):
                        evcopy(ci + ti, s_[0:mw, t0:t0 + tw], banks[acc[ti]][0:mw, 0:tw], [Bb[acc[ti]]], [sB])
                    kb.dma('sp', proj[col:col + mw, g0:g0 + T], s_[0:mw, :], sB, reads=[sB], writes=[B_proj], partial=True)

                groups = split_groups([(0, c.o_kpe), (c.o_kpe, 64), (c.o_qs, c.o_vs - c.o_qs), (c.o_ga, 2 * D)])
                gemm(st, wb, wbB, hn, hnB, DC, T, w_in[i], groups, evac)
                VW = c.HKV * 64
                w_, wB = wb[0], wbB[0]
                wv = w_[:, 0:DC * VW].rearrange("p (k n) -> p k n", n=VW)
                for k0 in range(0, DC, 16):
                    k1 = min(DC, k0 + 16)
                    kb.dma('pool', wv[:, k0:k1, :], w_in[i][k0 * 128:k1 * 128, c.o_vs:c.o_vs + VW].rearrange("(k p) n -> p k n", p=128), wB,
                           writes=[wB], partial=(k0 > 0))
                vst = [sb(st, "vst%d" % j, [128, VW], BF16) for j in range(2)]
                vstB = [Buf() for _ in range(2)]
                for tt in range((T + 127) // 128):
                    t0 = tt * 128
                    tw = min(128, T - t0)
                    bi = 6 + tt % 2
                    for kc in range(DC):
                        kb.op('pe', lambda e: e.matmul(banks[bi][0:tw, 0:VW], hn[:, kc, t0:t0 + tw], wv[:, kc, :], start=(kc == 0), stop=(kc == DC - 1)),
                              reads=[wB, hnB], writes=[Bb[bi]], signal=(kc == DC - 1))
                    evcopy(tt, vst[tt % 2][0:tw, :], banks[bi][0:tw, 0:VW], [Bb[bi]], [vstB[tt % 2]])
                    kb.dma('sp', vS[g0 + t0:g0 + t0 + tw, :], vst[tt % 2][0:tw, :], vstB[tt % 2], reads=[vstB[tt % 2]], writes=[B_vS], partial=True)
            kb.barrier()

        def rstd_parts(stats, parts, dim, tw, dst, dstB, bi):
            for pi, (src, srcB, kp) in enumerate(parts):
                sq, sqB = stats['sq'][pi % 2], stats['sqB'][pi % 2]
                kb.op('act', lambda e: e.activation(sq[0:kp, 0:tw], src, AF.Square), reads=srcB, writes=[sqB])
                kb.op('pe', lambda e: e.matmul(banks[bi][:, 0:tw], ones32[0:kp, :], sq[0:kp, 0:tw], start=(pi == 0), stop=(pi == len(parts) - 1)),
                      reads=[sqB, B_const], writes=[Bb[bi]])
            kb.op('act', lambda e: e.activation(dst[:, 0:tw], banks[bi][:, 0:tw], AF.Sqrt, bias=EPS, scale=1.0 / dim), reads=[Bb[bi]], writes=[dstB])
            kb.op('dve', lambda e: e.reciprocal(dst[:, 0:tw], dst[:, 0:tw]), reads=[dstB], writes=[dstB])

        def phase_B(i, g0, T):
            QC, KC4, HM = c.QR // 128, c.KVR // 128, c.HM
            with ExitStack() as st:
                stats = mk_stats(st, 512)
                cq = sb(st, "cq", [128, QC, T], BF16)
                ckv = sb(st, "ckv", [128, KC4, T], BF16)
                kpe = sb(st, "kpe", [64, T], BF16)
                cqB, ckvB, kpeB = Buf(), Buf(), Buf()
                kb.dma('sp', cq[:, :, :], proj[c.o_cq:c.o_cq + c.QR, g0:g0 + T].rearrange("(k p) t -> p k t", p=128), cqB, reads=[B_proj], writes=[cqB])
                kb.dma('sp', ckv[:, :, :], proj[c.o_ckv:c.o_ckv + c.KVR, g0:g0 + T].rearrange("(k p) t -> p k t", p=128), ckvB, reads=[B_proj], writes=[ckvB])
                kb.dma('sp', kpe[:, :], proj[c.o_kpe:c.o_kpe + 64, g0:g0 + T], kpeB, reads=[B_proj], writes=[kpeB])
                gcq, gcqB = col_vec(st, "gcq", cq_norm[i], 0, c.QR)
                gckv, gckvB = col_vec(st, "gckv", ckv_norm[i], 0, c.KVR)
                gqn, gqnB = col_vec(st, "gqn", q_norm[i], 0, 128)
                gqr, gqrB = col_vec(st, "gqr", q_norm[i], 128, 64, P=64)
                gkn, gknB = col_vec(st, "gkn", k_norm[i], 0, 128)
                gkr, gkrB = col_vec(st, "gkr", k_norm[i], 128, 64, P=64)
                cos = sb(st, "cos", [64, T], F32)
                sin = sb(st, "sin", [64, T], F32)
                prot = sb(st, "prot", [64, 64], F32)
                csB = Buf()
                kb.dma('sp', cos[:, :], c_cos[:, g0:g0 + T], csB, writes=[csB])
                kb.dma('sp', sin[:, :], c_sin[:, g0:g0 + T], csB, writes=[csB], partial=True)
                kb.dma('sp', prot[:, :], c_prot[:, :], csB, writes=[csB], partial=True)
                cqn = sb(st, "cqn", [128, QC, T], BF16)
                ckvn = sb(st, "ckvn", [128, KC4, T], BF16)
                cqnB, ckvnB = Buf(), Buf()
                R = sb(st, "R", [64, T], F32)
                RB = Buf()
                rr = sb(st, "rr", [128, 512], F32)
                rrB = Buf()
                y32 = sb(st, "y32", [64, 512], F32)
                yB = Buf()
                t1 = sb(st, "t1", [64, 512], F32)
                t1B = Buf()
                n32 = sb(st, "n32", [128, 512], F32)
                n32B = Buf()
                r32 = sb(st, "r32", [64, 512], F32)
                r32B = Buf()
                tcs = tchunks(T)
                for (t0, tw) in tcs:
                    rstd_parts(stats, [(cq[:, k, t0:t0 + tw], [cqB], 128) for k in range(QC)], c.QR, tw, rr, rrB, 6)
                    for k in range(QC):
                        kb.op('dve', lambda e: e.scalar_tensor_tensor(out=cqn[:, k, t0:t0 + tw], in0=cq[:, k, t0:t0 + tw], scalar=gcq[:, k:k + 1], in1=rr[:, 0:tw], op0=ALU.mult, op1=ALU.mult),
                              reads=[cqB, gcqB, rrB], writes=[cqnB])
                    rstd_parts(stats, [(ckv[:, k, t0:t0 + tw], [ckvB], 128) for k in range(KC4)], c.KVR, tw, rr, rrB, 6)
                    for k in range(KC4):
                        kb.op('dve', lambda e: e.scalar_tensor_tensor(out=ckvn[:, k, t0:t0 + tw], in0=ckv[:, k, t0:t0 + tw], scalar=gckv[:, k:k + 1], in1=rr[:, 0:tw], op0=ALU.mult, op1=ALU.mult),
                              reads=[ckvB, gckvB, rrB], writes=[ckvnB])
                    kb.op('dve', lambda e: e.tensor_scalar(out=y32[:, 0:tw], in0=kpe[:, t0:t0 + tw], scalar1=gkr[:, 0:1], scalar2=None, op0=ALU.mult), reads=[kpeB, gkrB], writes=[yB])
                    kb.op('pe', lambda e: e.matmul(banks[7][0:64, 0:tw], prot[:, :], y32[:, 0:tw], start=True, stop=True), reads=[yB, csB], writes=[Bb[7]])
                    kb.op('dve', lambda e: e.tensor_tensor(out=t1[:, 0:tw], in0=banks[7][0:64, 0:tw], in1=sin[:, t0:t0 + tw], op=ALU.mult), reads=[Bb[7], csB], writes=[t1B])
                    kb.op('dve', lambda e: e.tensor_tensor(out=y32[:, 0:tw], in0=y32[:, 0:tw], in1=cos[:, t0:t0 + tw], op=ALU.mult), reads=[yB, csB], writes=[yB])
                    kb.op('dve', lambda e: e.tensor_tensor(out=R[:, t0:t0 + tw], in0=y32[:, 0:tw], in1=t1[:, 0:tw], op=ALU.add), reads=[yB, t1B], writes=[RB])
                wq = [sb(st, "wq%d" % j, [128, QC, 192], BF16) for j in range(2)]
                wqB = [Buf() for _ in range(2)]
                wk = [sb(st, "wk%d" % j, [128, KC4, 128], BF16) for j in range(2)]
                wkB = [Buf() for _ in range(2)]
                so = [sb(st, "so%d" % j, [128, 512], BF16) for j in range(4)]
                soB = [Buf() for _ in range(4)]
                sk = 0
                it = 0
                for h in range(HM):
                    wq_, wqB_ = wq[h % 2], wqB[h % 2]
                    wk_, wkB_ = wk[h % 2], wkB[h % 2]
                    kb.dma('pool', wq_[:, :, :], w_uq[i][:, h * 192:(h + 1) * 192].rearrange("(k p) n -> p k n", p=128), wqB_, writes=[wqB_])
                    kb.dma('pool', wk_[:, :, :], w_ukv[i][:, h * 256:h * 256 + 128].rearrange("(k p) n -> p k n", p=128), wkB_, writes=[wkB_])
                    for (t0, tw) in tcs:
                        bA, bB, bS, bR = [(it % 2) * 4 + j for j in range(4)]
                        it += 1
                        for k in range(QC):
                            kb.op('pe', lambda e: e.matmul(banks[bA][:, 0:tw], wq_[:, k, 0:128], cqn[:, k, t0:t0 + tw], start=(k == 0), stop=(k == QC - 1)),
                                  reads=[wqB_, cqnB], writes=[Bb[bA]], signal=(k == QC - 1))
                        for k in range(QC):
                            kb.op('pe', lambda e: e.matmul(banks[bB][0:64, 0:tw], wq_[:, k, 128:192], cqn[:, k, t0:t0 + tw], start=(k == 0), stop=(k == QC - 1)),
                                  reads=[wqB_, cqnB], writes=[Bb[bB]], signal=(k == QC - 1))
                        kb.op('act', lambda e: e.copy(n32[:, 0:tw], banks[bA][:, 0:tw]), reads=[Bb[bA]], writes=[n32B])
                        kb.op('dve', lambda e: e.tensor_copy(r32[:, 0:tw], banks[bB][0:64, 0:tw]), reads=[Bb[bB]], writes=[r32B])
                        rstd_parts(stats, [(n32[:, 0:tw], [n32B], 128), (r32[:, 0:tw], [r32B], 64)], 192, tw, rr, rrB, bS)
                        s_, sB = so[sk % 4], soB[sk % 4]
                        sk += 1
                        kb.op('dve', lambda e: e.scalar_tensor_tensor(out=s_[:, 0:tw], in0=n32[:, 0:tw], scalar=gqn[:, 0:1], in1=rr[:, 0:tw], op0=ALU.mult, op1=ALU.mult),
                              reads=[n32B, gqnB, rrB], writes=[sB])
                        kb.dma('sp', qT[h * 192:h * 192 + 128, g0 + t0:g0 + t0 + tw], s_[:, 0:tw], sB, reads=[sB], writes=[B_qT], partial=True)
                        kb.op('dve', lambda e: e.scalar_tensor_tensor(out=y32[:, 0:tw], in0=r32[:, 0:tw], scalar=gqr[:, 0:1], in1=rr[0:64, 0:tw], op0=ALU.mult, op1=ALU.mult),
                              reads=[r32B, gqrB, rrB], writes=[yB])
                        kb.op('pe', lambda e: e.matmul(banks[bR][0:64, 0:tw], prot[:, :], y32[:, 0:tw], start=True, stop=True), reads=[yB, csB], writes=[Bb[bR]])
                        kb.op('dve', lambda e: e.tensor_tensor(out=t1[:, 0:tw], in0=banks[bR][0:64, 0:tw], in1=sin[:, t0:t0 + tw], op=ALU.mult), reads=[Bb[bR], csB], writes=[t1B])
                        kb.op('dve', lambda e: e.tensor_tensor(out=y32[:, 0:tw], in0=y32[:, 0:tw], in1=cos[:, t0:t0 + tw], op=ALU.mult), reads=[yB, csB], writes=[yB])
                        s_, sB = so[sk % 4], soB[sk % 4]
                        sk += 1
                        kb.op('dve', lambda e: e.tensor_tensor(out=s_[0:64, 0:tw], in0=y32[:, 0:tw], in1=t1[:, 0:tw], op=ALU.add), reads=[yB, t1B], writes=[sB])
                        kb.dma('sp', qT[h * 192 + 128:(h + 1) * 192, g0 + t0:g0 + t0 + tw], s_[0:64, 0:tw], sB, reads=[sB], writes=[B_qT], partial=True)
                        bA, bB, bS, bR = [(it % 2) * 4 + j for j in range(4)]
                        it += 1
                        for k in range(KC4):
                            kb.op('pe', lambda e: e.matmul(banks[bA][:, 0:tw], wk_[:, k, :], ckvn[:, k, t0:t0 + tw], start=(k == 0), stop=(k == KC4 - 1)),
                                  reads=[wkB_, ckvnB], writes=[Bb[bA]], signal=(k == KC4 - 1))
                        kb.op('act', lambda e: e.copy(n32[:, 0:tw], banks[bA][:, 0:tw]), reads=[Bb[bA]], writes=[n32B])
                        rstd_parts(stats, [(n32[:, 0:tw], [n32B], 128), (kpe[:, t0:t0 + tw], [kpeB], 64)], 192, tw, rr, rrB, bS)
                        s_, sB = so[sk % 4], soB[sk % 4]
                        sk += 1
                        kb.op('dve', lambda e: e.scalar_tensor_tensor(out=s_[:, 0:tw], in0=n32[:, 0:tw], scalar=gkn[:, 0:1], in1=rr[:, 0:tw], op0=ALU.mult, op1=ALU.mult),
                              reads=[n32B, gknB, rrB], writes=[sB])
                        kb.dma('sp', kT[h * 192:h * 192 + 128, g0 + t0:g0 + t0 + tw], s_[:, 0:tw], sB, reads=[sB], writes=[B_kT], partial=True)
                        s_, sB = so[sk % 4], soB[sk % 4]
                        sk += 1
                        kb.op('dve', lambda e: e.tensor_tensor(out=s_[0:64, 0:tw], in0=R[:, t0:t0 + tw], in1=rr[0:64, 0:tw], op=ALU.mult), reads=[RB, rrB], writes=[sB])
                        kb.dma('sp', kT[h * 192 + 128:(h + 1) * 192, g0 + t0:g0 + t0 + tw], s_[0:64, 0:tw], sB, reads=[sB], writes=[B_kT], partial=True)
                VW = HM * 128
                wv = sb(st, "wv", [128, KC4, HM, 128], BF16)
                wvB = Buf()
                for k in range(KC4):
                    kb.dma('pool', wv[:, k, :, :], w_ukv[i][k * 128:(k + 1) * 128, :].rearrange("p (h x) -> p h x", x=256)[:, :, 128:256], wvB, writes=[wvB], partial=(k > 0))
                wvf = wv[:, :, :, :].rearrange("p k h x -> p k (h x)")
                vst = [sb(st, "vst%d" % j, [128, VW], BF16) for j in range(2)]
                vstB = [Buf() for _ in range(2)]
                nb = (VW + 511) // 512
                for tt in range((T + 127) // 128):
                    t0 = tt * 128
                    tw = min(128, T - t0)
                    for j in range(nb):
                        cw = min(512, VW - j * 512)
                        bi = (tt % 2) * 4 + j
                        for k in range(KC4):
                            kb.op('pe', lambda e: e.matmul(banks[bi][0:tw, 0:cw], ckvn[:, k, t0:t0 + tw], wvf[:, k, j * 512:j * 512 + cw], start=(k == 0), stop=(k == KC4 - 1)),
                                  reads=[wvB, ckvnB], writes=[Bb[bi]], signal=(k == KC4 - 1))
                        evcopy(j, vst[tt % 2][0:tw, j * 512:j * 512 + cw], banks[bi][0:tw, 0:cw], [Bb[bi]], [vstB[tt % 2]] if j == 0 else [])
                        if j > 0:
                            ee = 'act' if j % 2 == 0 else 'dve'
                            vstB[tt % 2].w['E' + ee] = (kb.esem[ee], kb.ecnt[ee])
                    kb.dma('sp', vM[g0 + t0:g0 + t0 + tw, :], vst[tt % 2][0:tw, :], vstB[tt % 2], reads=[vstB[tt % 2]], writes=[B_vM], partial=True)
            kb.barrier()

        def phase_C(i, g0, T):
            HM = c.HM
            Lk = g0 + T
            nkb_all = (Lk + 127) // 128
            scale = 192.0 ** -0.5
            with ExitStack() as st:
                kn = [sb(st, "kn%d" % j, [128, Lk], BF16) for j in range(2)]
                kr = [sb(st, "kr%d" % j, [64, Lk], BF16) for j in range(2)]
                vh = [sb(st, "vh%d" % j, [128, nkb_all, 128], BF16) for j in range(2)]
                qn = [sb(st, "qn%d" % j, [128, T], BF16) for j in range(2)]
                qr = [sb(st, "qr%d" % j, [64, T], BF16) for j in range(2)]
                hB = [Buf() for _ in range(2)]
                pt = [sb(st, "pt%d" % j, [128, 512], BF16) for j in range(3)]
                ptB = [Buf() for _ in range(3)]
                rec = sb(st, "rec", [128, 512], F32)
                recB = Buf()
                ao = [sb(st, "ao%d" % j, [128, 512], BF16) for j in range(2)]
                aoB = [Buf() for _ in range(2)]
                pk = 0
                qk = 0
                for h in range(HM):
                    j = h % 2
                    B_ = hB[j]
                    kb.dma('sp', kn[j][:, :], kT[h * 192:h * 192 + 128, 0:Lk], B_, reads=[B_kT], writes=[B_])
                    kb.dma('sp', kr[j][:, :], kT[h * 192 + 128:(h + 1) * 192, 0:Lk], B_, reads=[B_kT], writes=[B_], partial=True)
                    nfull = Lk // 128
                    kb.dma('sp', vh[j][:, 0:nfull, :], vM[0:nfull * 128, h * 128:(h + 1) * 128].rearrange("(b p) d -> p b d", p=128), B_, reads=[B_vM], writes=[B_], partial=True)
                    if Lk % 128:
                        kb.dma('sp', vh[j][0:Lk % 128, nfull, :], vM[nfull * 128:Lk, h * 128:(h + 1) * 128], B_, reads=[B_vM], writes=[B_], partial=True)
                    kb.dma('sp', qn[j][:, :], qT[h * 192:h * 192 + 128, g0:g0 + T], B_, reads=[B_qT], writes=[B_], partial=True)
                    kb.dma('sp', qr[j][:, :], qT[h * 192 + 128:(h + 1) * 192, g0:g0 + T], B_, reads=[B_qT], writes=[B_], partial=True)
                    for qc0 in range(0, T, 512):
                        qw = min(512, T - qc0)
                        qa = g0 + qc0
                        nkb = (qa + qw + 127) // 128
                        bD, bO = 4 + (qk % 2) * 2, 5 + (qk % 2) * 2
                        for kbi in range(nkb):
                            k0 = kbi * 128
                            kw = min(128, Lk - k0)
                            delta = k0 - qa
                            qlo = max(0, delta)
                            n = qw - qlo
                            bS = pk % 2
                            p_, pB = pt[pk % 3], ptB[pk % 3]
                            pk += 1
                            kb.op('pe', lambda e: e.matmul(banks[bS][0:kw, 0:n], kn[j][:, k0:k0 + kw], qn[j][:, qc0 + qlo:qc0 + qw], start=True, stop=False),
                                  reads=[B_], writes=[Bb[bS]], signal=False)
                            kb.op('pe', lambda e: e.matmul(banks[bS][0:kw, 0:n], kr[j][:, k0:k0 + kw], qr[j][:, qc0 + qlo:qc0 + qw], start=False, stop=True),
                                  reads=[B_], writes=[Bb[bS]])
                            kb.op('act', lambda e: e.activation(p_[0:kw, 0:n], banks[bS][0:kw, 0:n], AF.Exp, scale=scale), reads=[Bb[bS]], writes=[pB])
                            if delta >= 0:
                                kb.op('dve', lambda e: e.tensor_tensor(out=p_[0:kw, 0:n], in0=p_[0:kw, 0:n], in1=tri[0:kw, 0:n], op=ALU.mult), reads=[pB, B_const], writes=[pB])
                            last = (kbi == nkb - 1)
                            kb.op('pe', lambda e: e.matmul(banks[bD][:, qlo:qw], onesbf[0:kw, :], p_[0:kw, 0:n], start=(kbi == 0), stop=last),
                                  reads=[pB, B_ones], writes=[Bb[bD]], signal=False)
                            kb.op('pe', lambda e: e.matmul(banks[bO][:, qlo:qw], vh[j][0:kw, kbi, :], p_[0:kw, 0:n], start=(kbi == 0), stop=last),
                                  reads=[pB, B_], writes=[Bb[bO]])
                        Bb[bD].w = dict(Bb[bO].w)
                        kb.op('dve', lambda e: e.reciprocal(rec[:, 0:qw], banks[bD][:, 0:qw]), reads=[Bb[bD]], writes=[recB])
                        a_, aB = ao[qk % 2], aoB[qk % 2]
                        kb.op('dve', lambda e: e.tensor_tensor(out=a_[:, 0:qw], in0=banks[bO][:, 0:qw], in1=rec[:, 0:qw], op=ALU.mult), reads=[Bb[bO], recB], writes=[aB])
                        kb.dma('sp', aT[h * 128:(h + 1) * 128, qa:qa + qw], a_[:, 0:qw], aB, reads=[aB], writes=[B_aT], partial=True)
                        qk += 1
            kb.barrier()


        def phase_E0():
            HS = c.HS
            with ExitStack() as st:
                oh = sb(st, "oh", [32, 128], F32)
                rb = sb(st, "rb", [32, HS], F32)
                anti = sb(st, "anti", [128, 384], F32)
                tb = sb(st, "tb", [128, HS], F32)
                ntri = sb(st, "ntri", [128, 128], BF16)
                lB, tbB, nB = Buf(), Buf(), Buf()
                kb.dma('sp', oh[:, :], c_oh[:, :], lB, writes=[lB])
                kb.dma('sp', rb[:, :], rel_bias[:, :], lB, writes=[lB], partial=True)
                kb.dma('sp', anti[:, :], c_anti[:, :], lB, writes=[lB], partial=True)
                kb.op('pe', lambda e: e.matmul(banks[0][:, 0:HS], oh[:, :], rb[:, :], start=True, stop=True), reads=[lB], writes=[Bb[0]])
                kb.op('act', lambda e: e.copy(tb[:, :], banks[0][:, 0:HS]), reads=[Bb[0]], writes=[tbB])
                kb.op('dve', lambda e: e.tensor_scalar(out=ntri[:, :], in0=tri[:, 0:128], scalar1=-1.0, scalar2=1.0, op0=ALU.mult, op1=ALU.add), reads=[B_const], writes=[nB])
                for wi, (dst, msk, base) in enumerate(((Ecur_d, tri, 255), (Eprev_d, ntri, 127))):
                    E = sb(st, "E%d" % wi, [128, HS, 128], BF16)
                    EB = Buf()
                    QB = 512 // HS
                    for qb in range(0, 128, QB):
                        bi = 1 + (qb // QB) % 7
                        for q in range(qb, qb + QB):
                            sc = base - q
                            kb.op('pe', lambda e: e.matmul(banks[bi][:, (q - qb) * HS:(q - qb + 1) * HS], anti[:, sc:sc + 128], tb[:, :], start=True, stop=True),
                                  reads=[lB, tbB], writes=[Bb[bi]], signal=(q == qb + QB - 1))
                        kb.op('act', lambda e: e.activation(E[:, :, qb:qb + QB].rearrange("k h q -> k q h"), banks[bi][:, 0:QB * HS].rearrange("k (q h) -> k q h", h=HS), AF.Exp),
                              reads=[Bb[bi]], writes=[EB])
                    for h in range(HS):
                        kb.op('dve', lambda e: e.tensor_tensor(out=E[:, h, :], in0=E[:, h, :], in1=msk[:, 0:128], op=ALU.mult), reads=[EB, nB, B_const], writes=[EB])
                    kb.dma('sp', dst[:, :], E[:, :, :].rearrange("k h q -> k (h q)"), EB, reads=[EB], writes=[B_E], partial=True)
            kb.barrier()

        def phase_D(i, g0, T):
            HS, HKV = c.HS, c.HKV
            k00 = max(0, g0 - 128)
            Tk = g0 + T - k00
            nblk = (Tk + 127) // 128
            with ExitStack() as st:
                stats = mk_stats(st, 512)
                Ec = sb(st, "Ec", [128, HS, 128], BF16)
                Ep = sb(st, "Ep", [128, HS, 128], BF16)
                EB = Buf()
                kb.dma('sp', Ec[:, :, :].rearrange("k h q -> k (h q)"), Ecur_d[:, :], EB, reads=[B_E], writes=[EB])
                kb.dma('sp', Ep[:, :, :].rearrange("k h q -> k (h q)"), Eprev_d[:, :], EB, reads=[B_E], writes=[EB], partial=True)
                qs = sb(st, "qs", [64, HS, T], BF16)
                ks = sb(st, "ks", [64, HKV, Tk], BF16)
                vs = sb(st, "vs", [128, nblk, HKV * 64], BF16)
                qB, kB_, vB = Buf(), Buf(), Buf()
                kb.dma('sp', qs[:, :, :], proj[c.o_qs:c.o_qs + HS * 64, g0:g0 + T].rearrange("(h d) t -> d h t", d=64), qB, reads=[B_proj], writes=[qB])
                kb.dma('sp', ks[:, :, :], proj[c.o_ks:c.o_ks + HKV * 64, k00:k00 + Tk].rearrange("(h d) t -> d h t", d=64), kB_, reads=[B_proj], writes=[kB_])
                nfull = Tk // 128
                kb.dma('sp', vs[:, 0:nfull, :], vS[k00:k00 + nfull * 128, :].rearrange("(b p) f -> p b f", p=128), vB, reads=[B_vS], writes=[vB])
                if Tk % 128:
                    kb.dma('sp', vs[0:Tk % 128, nfull, :], vS[k00 + nfull * 128:k00 + Tk, :], vB, reads=[B_vS], writes=[vB], partial=True)
                gq, gqB = col_vec(st, "gsq", sq_norm[i], 0, 64, P=64)
                gk, gkB = col_vec(st, "gsk", sk_norm[i], 0, 64, P=64)
                sk1 = sb(st, "sk1", [1, HS], F32)
                skB = Buf()
                kb.dma('sp', sk1[:, :], sinks[i:i + 1, :], skB, writes=[skB])
                kb.op('act', lambda e: e.activation(sk1[:, :], sk1[:, :], AF.Exp), reads=[skB], writes=[skB])
                kb.op('pe', lambda e: e.matmul(banks[7][0:64, 0:HS], ones32[0:1, 0:64], sk1[0:1, :], start=True, stop=True), reads=[skB, B_const], writes=[Bb[7]])
                sE = sb(st, "sE", [64, HS], F32)
                sEB = Buf()
                kb.op('act', lambda e: e.copy(sE[:, :], banks[7][0:64, 0:HS]), reads=[Bb[7]], writes=[sEB])
                rr = sb(st, "rrs", [128, 512], F32)
                rrB = Buf()
                for (t_, tB_, nh, TT, g_, gB_) in ((qs, qB, HS, T, gq, gqB), (ks, kB_, HKV, Tk, gk, gkB)):
                    for h in range(nh):
                        for (t0, tw) in tchunks(TT):
                            rstd_parts(stats, [(t_[:, h, t0:t0 + tw], [tB_], 64)], 64, tw, rr, rrB, 6)
                            kb.op('dve', lambda e: e.scalar_tensor_tensor(out=t_[:, h, t0:t0 + tw], in0=t_[:, h, t0:t0 + tw], scalar=g_[:, 0:1], in1=rr[0:64, 0:tw], op0=ALU.mult, op1=ALU.mult),
                                  reads=[tB_, gB_, rrB], writes=[tB_])
                pt = [sb(st, "spt%d" % j, [128, 4, 128], BF16) for j in range(4)]
                ptB = [Buf() for _ in range(4)]
                dd = sb(st, "dd", [64, 4, 128], F32)
                ddB = Buf()
                bo = [sb(st, "bo%d" % j, [64, 4, 128], BF16) for j in range(2)]
                boB = [Buf() for _ in range(2)]
                grp = HS // HKV
                assert grp == 4
                it = 0
                pk = 0
                for jb in range((T + 127) // 128):
                    q0 = jb * 128
                    qw = min(128, T - q0)
                    qabs = g0 + q0
                    for kvh in range(HKV):
                        bD, bO = 4 + (it % 2) * 2, 5 + (it % 2) * 2
                        blocks = []
                        if qabs >= 128:
                            blocks.append((Ep, qabs - 128 - k00, 128))
                        blocks.append((Ec, qabs - k00, qw))
                        for bi_, (E_, kc0, kw) in enumerate(blocks):
                            bS = pk % 4
                            p_, pB = pt[pk % 4], ptB[pk % 4]
                            pk += 1
                            Sv = banks[bS][0:kw, :].rearrange("p (h q) -> p h q", h=4)[:, :, 0:qw]
                            kb.op('pe', lambda e: e.matmul(Sv, ks[:, kvh, kc0:kc0 + kw], qs[:, kvh * 4:kvh * 4 + 4, q0:q0 + qw], start=True, stop=True),
                                  reads=[kB_, qB], writes=[Bb[bS]])
                            kb.op('act', lambda e: e.activation(p_[0:kw, :, 0:qw], Sv, AF.Exp, scale=0.125), reads=[Bb[bS]], writes=[pB])
                            kb.op('dve', lambda e: e.tensor_tensor(out=p_[0:kw, :, 0:qw], in0=p_[0:kw, :, 0:qw], in1=E_[0:kw, kvh * 4:kvh * 4 + 4, 0:qw], op=ALU.mult), reads=[pB, EB], writes=[pB])
                            blk = kc0 // 128
                            Dv = banks[bD][0:64, :].rearrange("p (h q) -> p h q", h=4)[:, :, 0:qw]
                            Ov = banks[bO][0:64, :].rearrange("p (h q) -> p h q", h=4)[:, :, 0:qw]
                            kb.op('pe', lambda e: e.matmul(Dv, onesbf[0:kw, 0:64], p_[0:kw, :, 0:qw], start=(bi_ == 0), stop=(bi_ == len(blocks) - 1)),
                                  reads=[pB, B_ones], writes=[Bb[bD]], signal=False)
                            kb.op('pe', lambda e: e.matmul(Ov, vs[0:kw, blk, kvh * 64:(kvh + 1) * 64], p_[0:kw, :, 0:qw], start=(bi_ == 0), stop=(bi_ == len(blocks) - 1)),
                                  reads=[pB, vB], writes=[Bb[bO]])
                        Bb[bD].w = dict(Bb[bO].w)
                        for hh in range(4):
                            kb.op('dve', lambda e: e.tensor_scalar(out=dd[:, hh, 0:qw], in0=Dv[:, hh, :], scalar1=sE[:, kvh * 4 + hh:kvh * 4 + hh + 1], scalar2=None, op0=ALU.add),
                                  reads=[Bb[bD], sEB], writes=[ddB])
                        kb.op('dve', lambda e: e.reciprocal(dd[:, :, 0:qw], dd[:, :, 0:qw]), reads=[ddB], writes=[ddB])
                        b_, bB = bo[it % 2], boB[it % 2]
                        kb.op('dve', lambda e: e.tensor_tensor(out=b_[:, :, 0:qw], in0=Ov, in1=dd[:, :, 0:qw], op=ALU.mult), reads=[Bb[bO], ddB], writes=[bB])
                        kb.dma('sp', bT[kvh * 256:(kvh + 1) * 256, qabs:qabs + qw].rearrange("(h d) t -> d h t", d=64), b_[:, :, 0:qw], bB, reads=[bB], writes=[B_bT], partial=True)
                        it += 1
            kb.barrier()

        def phase_F(i, g0, T):
            KA, KS = c.HM, c.HS // 2
            with ExitStack() as st:
                a_ = sb(st, "a_in", [128, KA, T], BF16)
                b_ = sb(st, "b_in", [128, KS, T], BF16)
                abB = Buf()
                kb.dma('sp', a_[:, :, :], aT[:, g0:g0 + T].rearrange("(k p) t -> p k t", p=128), abB, reads=[B_aT], writes=[abB])
                kb.dma('sp', b_[:, :, :], bT[:, g0:g0 + T].rearrange("(k p) t -> p k t", p=128), abB, reads=[B_bT], writes=[abB], partial=True)
                wb = [sb(st, "wbf%d" % j, [128, WB], BF16) for j in range(2)]
                wbB = [Buf() for _ in range(2)]
                ga = [sb(st, "ga%d" % j, [128, T], BF16) for j in range(2)]
                gb = [sb(st, "gb%d" % j, [128, T], BF16) for j in range(2)]
                gB = [Buf() for _ in range(2)]
                sa = sb(st, "sa", [128, T], F32)
                sbb = sb(st, "sbb", [128, T], F32)
                saB, sbB = Buf(), Buf()
                stg = [sb(st, "mstg%d" % j, [128, T], BF16) for j in range(2)]
                stgB = [Buf() for _ in range(2)]
                tcs = tchunks(T)

                def evac(ci, col, mw, accA, accB):
                    g_, gB_ = ci % 2, gB[ci % 2]
                    kb.dma('sp', ga[g_][:, :], proj[c.o_ga + col:c.o_ga + col + 128, g0:g0 + T], gB_, reads=[B_proj], writes=[gB_])
                    kb.dma('sp', gb[g_][:, :], proj[c.o_gb + col:c.o_gb + col + 128, g0:g0 + T], gB_, reads=[B_proj], writes=[gB_], partial=True)
                    kb.op('act', lambda e: e.activation(sa[:, :], ga[g_][:, :], AF.Sigmoid), reads=[gB_], writes=[saB])
                    kb.op('act', lambda e: e.activation(sbb[:, :], gb[g_][:, :], AF.Sigmoid), reads=[gB_], writes=[sbB])
                    s_, sB = stg[ci % 2], stgB[ci % 2]
                    for ti, (t0, tw) in enumerate(tcs):
                        kb.op('dve', lambda e: e.tensor_tensor(out=sa[:, t0:t0 + tw], in0=banks[accA[ti]][:, 0:tw], in1=sa[:, t0:t0 + tw], op=ALU.mult), reads=[Bb[accA[ti]], saB], writes=[saB])
                        kb.op('dve', lambda e: e.tensor_tensor(out=sbb[:, t0:t0 + tw], in0=banks[accB[ti]][:, 0:tw], in1=sbb[:, t0:t0 + tw], op=ALU.mult), reads=[Bb[accB[ti]], sbB], writes=[sbB])
                    kb.op('dve', lambda e: e.tensor_tensor(out=s_[:, :], in0=sa[:, :], in1=sbb[:, :], op=ALU.add), reads=[saB, sbB], writes=[sB])
                    kb.dma('sp', mg[col:col + 128, g0:g0 + T], s_[:, :], sB, reads=[sB], writes=[B_mg], partial=True)

                gw = 256 if (KA + KS) * 256 <= WB else 128
                gemm(st, wb, wbB, a_, abB, KA, T, w_bm[i], split_groups([(0, D)], gw), evac, xs2=b_, W2d2=w_bs[i], KC2=KS)
            kb.barrier()

        def resid_evac(st, T, g0):
            hr = [sb(st, "hr%d" % j, [128, T], F32) for j in range(2)]
            hrB = [Buf() for _ in range(2)]
            tcs = tchunks(T)

            def evac(ci, col, mw, acc, _, gate=None):
                h_, hB_ = hr[ci % 2], hrB[ci % 2]
                kb.dma('sp', h_[:, :], hres[col:col + 128, g0:g0 + T], hB_, reads=[B_hres], writes=[hB_])
                for ti, (t0, tw) in enumerate(tcs):
                    if gate is None:
                        kb.op('dve', lambda e: e.tensor_tensor(out=h_[:, t0:t0 + tw], in0=banks[acc[ti]][:, 0:tw], in1=h_[:, t0:t0 + tw], op=ALU.add), reads=[Bb[acc[ti]], hB_], writes=[hB_])
                    else:
                        gt_, gtB_ = gate
                        kb.op('dve', lambda e: e.tensor_tensor(out=banks[acc[ti]][:, 0:tw], in0=banks[acc[ti]][:, 0:tw], in1=gt_[:, t0:t0 + tw], op=ALU.mult), reads=[Bb[acc[ti]], gtB_], writes=[Bb[acc[ti]]])
                        kb.op('dve', lambda e: e.tensor_tensor(out=h_[:, t0:t0 + tw], in0=banks[acc[ti]][:, 0:tw], in1=h_[:, t0:t0 + tw], op=ALU.add), reads=[Bb[acc[ti]], hB_], writes=[hB_])
                kb.dma('sp', hres[col:col + 128, g0:g0 + T], h_[:, :], hB_, reads=[hB_], writes=[B_hres])
            return evac

        def phase_G(i, g0, T):
            with ExitStack() as st:
                m_ = sb(st, "m_in", [128, DC, T], BF16)
                mB = Buf()
                kb.dma('sp', m_[:, :, :], mg[:, g0:g0 + T].rearrange("(k p) t -> p k t", p=128), mB, reads=[B_mg], writes=[mB])
                wb = [sb(st, "wbg%d" % j, [128, WB], BF16) for j in range(2)]
                wbB = [Buf() for _ in range(2)]
                gemm(st, wb, wbB, m_, mB, DC, T, w_out[i], split_groups([(0, D)], 512), resid_evac(st, T, g0))
            kb.barrier()

        def phase_H(i, f0, T):
            moe = (i % 2 == 1)
            FFd = c.EFF if moe else c.FF
            FC = FFd // 128
            tcs = tchunks(T)
            with ExitStack() as st:
                stats = mk_stats(st, T)
                hn = sb(st, "hnf", [128, DC, T], BF16)
                hnB = Buf()
                gates = None
                if moe:
                    NE = c.NE
                    rt = sb(st, "rt", [128, DC, NE], F32)
                    rtB = Buf()
                    kb.dma('sp', rt[:, :, :], router[i // 2].rearrange("(k p) e -> p k e", p=128), rtB, writes=[rtB])
                    hf = [sb(st, "hf%d" % j, [128, T], F32) for j in range(2)]
                    hfB = [Buf() for _ in range(2)]
                    ntt = (T + 127) // 128

                    def f32cb(cc, xx, xB, gt, gtB, rstd, rB):
                        h_, hB_ = hf[cc % 2], hfB[cc % 2]
                        kb.op('pool', lambda e: e.scalar_tensor_tensor(out=h_[:, :], in0=xx[:, :], scalar=gt[:, cc:cc + 1], in1=rstd[:, :], op0=ALU.mult, op1=ALU.mult) if False else e.tensor_tensor(out=h_[:, :], in0=xx[:, :], in1=rstd[:, :], op=ALU.mult),
                              reads=[xB, rB], writes=[hB_])
                        kb.op('pool', lambda e: e.tensor_scalar(out=h_[:, :], in0=h_[:, :], scalar1=gt[:, cc:cc + 1], scalar2=None, op0=ALU.mult), reads=[hB_, gtB], writes=[hB_])
                        for tt in range(ntt):
                            t0 = tt * 128
                            tw = min(128, T - t0)
                            kb.op('pe', lambda e: e.matmul(banks[7][0:tw, tt * NE:(tt + 1) * NE], h_[:, t0:t0 + tw], rt[:, cc, :], start=(cc == 0 and tt == 0), stop=(cc == DC - 1 and tt == ntt - 1), skip_group_check=True),
                                  reads=[hB_, rtB], writes=[Bb[7]], signal=(tt == ntt - 1))
                    norm_load(st, stats, ffn_norm[i], f0, T, hn, hnB, f32cb=f32cb)
                    lg = sb(st, "lg", [128, ntt, NE], F32)
                    mx = sb(st, "mx", [128, ntt, 8], F32)
                    gtm = sb(st, "gtm", [128, ntt, NE], F32)
                    den = sb(st, "den", [128, ntt], F32)
                    lgB = Buf()
                    kb.op('dve', lambda e: e.memset(lg[:, :, :], -30000.0), writes=[lgB])
                    for tt in range(ntt):
                        tw = min(128, T - tt * 128)
                        kb.op('dve', lambda e: e.tensor_copy(lg[0:tw, tt, :], banks[7][0:tw, tt * NE:(tt + 1) * NE]), reads=[Bb[7], lgB], writes=[lgB])
                    for tt in range(ntt):
                        kb.op('dve', lambda e: e.max(mx[:, tt, :], lg[:, tt, :]), reads=[lgB], writes=[lgB])
                    for tt in range(ntt):
                        kb.op('dve', lambda e: e.tensor_scalar(out=gtm[:, tt, :], in0=lg[:, tt, :], scalar1=mx[:, tt, 0:1], scalar2=None, op0=ALU.subtract), reads=[lgB], writes=[lgB])
                        kb.op('act', lambda e: e.activation(gtm[:, tt, :], gtm[:, tt, :], AF.Exp), reads=[lgB], writes=[lgB])
                        kb.op('dve', lambda e: e.tensor_scalar(out=lg[:, tt, :], in0=lg[:, tt, :], scalar1=mx[:, tt, 1:2], scalar2=None, op0=ALU.is_ge), reads=[lgB], writes=[lgB])
                        kb.op('dve', lambda e: e.tensor_tensor(out=gtm[:, tt, :], in0=gtm[:, tt, :], in1=lg[:, tt, :], op=ALU.mult), reads=[lgB], writes=[lgB])
                        kb.op('dve', lambda e: e.tensor_tensor(out=den[:, tt:tt + 1], in0=mx[:, tt, 1:2], in1=mx[:, tt, 0:1], op=ALU.subtract), reads=[lgB], writes=[lgB])
                        kb.op('act', lambda e: e.activation(den[:, tt:tt + 1], den[:, tt:tt + 1], AF.Exp), reads=[lgB], writes=[lgB])
                        kb.op('dve', lambda e: e.tensor_scalar(out=den[:, tt:tt + 1], in0=den[:, tt:tt + 1], scalar1=1.0, scalar2=None, op0=ALU.add), reads=[lgB], writes=[lgB])
                        kb.op('dve', lambda e: e.reciprocal(den[:, tt:tt + 1], den[:, tt:tt + 1]), reads=[lgB], writes=[lgB])
                        kb.op('dve', lambda e: e.tensor_scalar(out=gtm[:, tt, :], in0=gtm[:, tt, :], scalar1=den[:, tt:tt + 1], scalar2=None, op0=ALU.mult), reads=[lgB], writes=[lgB])
                    gT = sb(st, "gT", [NE, T], F32)
                    gTB = Buf()
                    for tt in range(ntt):
                        t0 = tt * 128
                        tw = min(128, T - t0)
                        kb.op('pe', lambda e: e.transpose(banks[6][0:NE, 0:tw], gtm[0:tw, tt, :], ident[0:tw, 0:tw]), reads=[lgB, B_const], writes=[Bb[6]])
                        kb.op('act', lambda e: e.copy(gT[:, t0:t0 + tw], banks[6][0:NE, 0:tw]), reads=[Bb[6]], writes=[gTB])
                    sel = sb(st, "sel", [NE, NE, 128], F32)
                    selB = Buf()
                    kb.op('dve', lambda e: e.memset(sel[:, :, :], 0.0), writes=[selB])
                    for e_ in range(NE):
                        kb.op('dve', lambda e: e.tensor_scalar(out=sel[:, e_, :], in0=sel[:, e_, :], scalar1=ident[0:NE, e_:e_ + 1], scalar2=None, op0=ALU.add), reads=[selB, B_const], writes=[selB])
                    gbc = sb(st, "gbc", [128, NE, T], F32)
                    gbcB = Buf()
                    for e_ in range(NE):
                        for ti, (t0, tw) in enumerate(tcs):
                            kb.op('pe', lambda e: e.matmul(banks[ti][:, 0:tw], sel[:, e_, :], gT[:, t0:t0 + tw], start=True, stop=True), reads=[selB, gTB], writes=[Bb[ti]])
                            kb.op('act', lambda e: e.copy(gbc[:, e_, t0:t0 + tw], banks[ti][:, 0:tw]), reads=[Bb[ti]], writes=[gbcB])
                    gates = (gbc, gbcB)
                else:
                    norm_load(st, stats, ffn_norm[i], f0, T, hn, hnB)
                hid = sb(st, "hid", [128, FC, T], BF16)
                hidB = Buf()
                if 'dbg_hn' in dbg:
                    kb.dma('sp', dbg_hn[:, f0:f0 + T].rearrange("(k p) t -> p k t", p=128), hn[:, :, :], hnB, reads=[hnB], writes=[Buf()])
                WBH = 16384
                wb = [sb(st, "wbh%d" % j, [128, WBH], BF16) for j in range(2)]
                wbB = [Buf() for _ in range(2)]
                sl = sb(st, "sl", [128, T], F32)
                slB = Buf()
                revac = resid_evac(st, T, f0)
                for e_ in range(c.NE if moe else 1):
                    W1 = m_w1[i // 2, e_] if moe else d_w1[i // 2]
                    W3 = m_w3[i // 2, e_] if moe else d_w3[i // 2]
                    W2 = m_w2[i // 2, e_] if moe else d_w2[i // 2]

                    def evac13(ci, col, mw, accA, accB):
                        j = col // 128
                        for ti, (t0, tw) in enumerate(tcs):
                            kb.op('act', lambda e: e.activation(sl[:, t0:t0 + tw], banks[accA[ti]][:, 0:tw], AF.Sigmoid), reads=[Bb[accA[ti]]], writes=[slB])
                            kb.op('dve', lambda e: e.tensor_tensor(out=sl[:, t0:t0 + tw], in0=banks[accA[ti]][:, 0:tw], in1=sl[:, t0:t0 + tw], op=ALU.mult), reads=[Bb[accA[ti]], slB], writes=[slB])
                            kb.op('dve', lambda e: e.tensor_tensor(out=hid[:, j, t0:t0 + tw], in0=banks[accB[ti]][:, 0:tw], in1=sl[:, t0:t0 + tw], op=ALU.mult), reads=[Bb[accB[ti]], slB], writes=[hidB])
                    gw13 = 256 if 2 * DC * 256 <= WBH else 128
                    gemm(st, wb, wbB, hn, hnB, DC, T, W1, split_groups([(0, FFd)], gw13), evac13, xs2=hn, W2d2=W3, KC2=DC)
                    if 'dbg_hid' in dbg and e_ == 0:
                        kb.dma('sp', dbg_hid[0:FFd, f0:f0 + T].rearrange("(k p) t -> p k t", p=128), hid[:, :, :], hidB, reads=[hidB], writes=[Buf()])
                    if moe:
                        def evac2(ci, col, mw, acc, _, e__=e_):
                            revac(ci, col, mw, acc, None, gate=(gates[0][:, e__, :], gates[1]))
                    else:
                        evac2 = revac
                    gw2 = 256 if FC * 256 <= WBH else 128
                    gemm(st, wb, wbB, hid, hidB, FC, T, W2, split_groups([(0, D)], gw2), evac2)
            kb.barrier()

        def phase_out():
            with ExitStack() as st:
                ht = [sb(st, "ht%d" % i, [128, 4, 128], F32) for i in range(2)]
                htB = [Buf() for _ in range(2)]
                ot = [sb(st, "oo%d" % i, [128, D], F32) for i in range(2)]
                otB = [Buf() for _ in range(2)]
                k = 0
                nt = c.S // 128
                for tt in range(nt):
                    t0 = c.NM + tt * 128
                    o, oB = ot[tt % 2], otB[tt % 2]
                    for c4 in range(DC // 4):
                        h_, hB = ht[k % 2], htB[k % 2]
                        kb.dma('sp', h_[:, :, :], hres[c4 * 512:(c4 + 1) * 512, t0:t0 + 128].rearrange("(j p) t -> p j t", p=128), hB,
                               reads=[B_hres], writes=[hB])
                        bi = k % 8
                        for j in range(4):
                            kb.op('pe', lambda e: e.transpose(banks[bi][:, j * 128:(j + 1) * 128], h_[:, j, :], ident[:, :]),
                                  reads=[hB, B_const], writes=[Bb[bi]] if j == 0 else [], signal=(j == 3))
                        if k % 2 == 0:
                            kb.op('act', lambda e: e.copy(o[:, c4 * 512:(c4 + 1) * 512], banks[bi][:, :]), reads=[Bb[bi]], writes=[oB] if c4 == 0 else [])
                        else:
                            kb.op('dve', lambda e: e.tensor_copy(o[:, c4 * 512:(c4 + 1) * 512], banks[bi][:, :]), reads=[Bb[bi]], writes=[oB] if c4 == 0 else [])
                        if c4 > 0:
                            ee = 'act' if k % 2 == 0 else 'dve'
                            oB.w['E' + ee] = (kb.esem[ee], kb.ecnt[ee])
                        k += 1
                    kb.dma('sp', out[tt * 128:(tt + 1) * 128, :], o[:, :], oB, reads=[oB], writes=[B_out], partial=True)
            kb.barrier()

        nl = getattr(c, 'nlayers_run', c.DEPTH)
        if 'D' in getattr(c, 'stages', 'ABCDEFGH'):
            phase_E0()
        stages = getattr(c, 'stages', 'ABCDEFGH')
        for i in range(nl):
            for ph, fn in (('A', phase_A), ('B', phase_B), ('C', phase_C), ('D', phase_D), ('F', phase_F), ('G', phase_G)):
                if ph in stages:
                    for (g0, T) in c.groups:
                        fn(i, g0, T)
            if 'H' in stages:
                f0 = 0
                while f0 < L:
                    T = min(c.FT, L - f0)
                    phase_H(i, f0, T)
                    f0 += T
        phase_out()
    return nc


def make_consts(cfg):
    L = cfg.L
    ident = np.eye(128, dtype=np.float32)
    tri = (np.arange(512)[None, :] >= np.arange(128)[:, None]).astype(np.float32)
    half = 32
    inv_freq = (10000.0 ** (-np.arange(half, dtype=np.float32) / half)).astype(np.float32)
    ang = np.arange(L, dtype=np.float32)[None, :] * inv_freq[:, None]
    cos = np.cos(ang).astype(np.float32)
    sin = np.sin(ang).astype(np.float32)
    cos2 = np.concatenate([cos, cos], axis=0)
    sin2 = np.concatenate([sin, sin], axis=0)
    prot = np.zeros((64, 64), np.float32)
    for m in range(32):
        prot[m + 32, m] = -1.0
        prot[m, m + 32] = 1.0
    rel = np.arange(128)
    n = np.maximum(rel, 0)
    nf = np.maximum(n, 1).astype(np.float32)
    large = 16 + (np.log(nf / 16) / math.log(128 / 16) * 16).astype(np.int32)
    large = np.minimum(large, 31)
    bucket = np.where(n < 16, n, large)
    oh = np.zeros((32, 128), np.float32)
    oh[bucket, rel] = 1.0
    anti = np.zeros((128, 384), np.float32)
    for r in range(128):
        anti[r, 255 - r] = 1.0
    return {"c_ident": ident, "c_tri": tri, "c_cos": cos2, "c_sin": sin2, "c_prot": prot, "c_oh": oh, "c_anti": anti}


WNAMES = ["meta_tokens", "rel_bias", "attn_norm", "w_in", "mla_cq_norm", "mla_ckv_norm", "mla_w_uq", "mla_w_ukv",
          "mla_q_norm", "mla_k_norm", "swa_q_norm", "swa_k_norm", "swa_sinks", "w_branch_mla", "w_branch_swa", "w_out",
          "ffn_norm", "dense_w1", "dense_w3", "dense_w2", "moe_router", "moe_w1", "moe_w3", "moe_w2"]


def run(cfg, inputs, dbg=()):
    nc = build(cfg, dbg)
    consts = make_consts(cfg)
    in_maps = []
    for b in range(cfg.B):
        m = {"x": np.ascontiguousarray(inputs["x"][b], dtype=np.float32)}
        for n in WNAMES:
            m[n] = np.asarray(inputs[n], dtype=np.float32)
        m.update(consts)
        in_maps.append(m)
    res = run_bass_kernel_spmd(nc, in_maps, core_ids=list(range(cfg.B)))
    return res


def kernel(**inputs):
    cfg = Cfg()
    res = run(cfg, inputs)
    return np.stack([res.results[b]["out"] for b in range(cfg.B)], axis=0).astype(np.float32)
```
